# Optimizing a Trainium2 kernel written in Bass

```python
import jax
import jax.numpy as jnp
from jax import lax
import numpy as np

D_MODEL = 1024
BATCH = 4
SEQ = 4096
DEPTH = 2

GRID_W = 64
CTX_LEN = 256
N_GROUPS = 4
GROUP_W = D_MODEL // N_GROUPS
EPS = 1e-6

FNET_GROUPS = 4
FNET_CH = GROUP_W // FNET_GROUPS

LRU_HEADS = 4
LRU_HEAD_DIM = GROUP_W // LRU_HEADS
LRU_CONV = 4
LRU_C = 8.0

MLA_HEADS = 4
MLA_NOPE = 64
MLA_ROPE = 32
MLA_V = GROUP_W // MLA_HEADS
MLA_Q_LORA = 256
MLA_KV_LORA = 128
ROPE_BASE = 10000.0
Q_BLOCK = 128
ATTN_SCALE = (MLA_NOPE + MLA_ROPE) ** -0.5

SC_CONV = 3

N_EXPERTS = 32
TOP_K = 4
D_EXPERT = D_MODEL
SWIGLU_ALPHA = 1.702
SWIGLU_LIMIT = 7.0
MOE_BLOCK = 128

PROJ_SIZES = (GROUP_W, GROUP_W, GROUP_W, MLA_Q_LORA, MLA_KV_LORA, MLA_ROPE, GROUP_W, GROUP_W, GROUP_W)
P_TOTAL = 6 * GROUP_W + MLA_Q_LORA + MLA_KV_LORA + MLA_ROPE

kernel_name = 'hybrid_parallel_group_diffusion_block'


def rms_norm(x, g):
    x32 = x.astype(jnp.float32)
    y = x32 * lax.rsqrt(jnp.mean(x32 * x32, axis=-1, keepdims=True) + EPS)
    return (y * g.astype(jnp.float32)).astype(x.dtype)


def modulate(h, shift, scale):
    return h * (1 + scale) + shift


def split_projection(p):
    idx = np.cumsum(PROJ_SIZES)[:-1].tolist()
    return jnp.split(p, idx, axis=-1)


def depthwise_conv(u, w, b, left):
    k_w = w.shape[0]
    n = u.shape[1]
    up = jnp.pad(u, ((0, 0), (left, k_w - 1 - left), (0, 0)))
    return sum(w[k] * up[:, k:k + n] for k in range(k_w)) + b


def fourier_mix(u):
    bsz, n, _ = u.shape
    ug = u.astype(jnp.float32).reshape(bsz, n, FNET_GROUPS, FNET_CH)
    y = jnp.fft.fft2(ug, axes=(1, 3), norm='ortho').real
    return y.reshape(bsz, n, GROUP_W).astype(u.dtype)


def _linear_combine(e1, e2):
    a1, b1 = e1
    a2, b2 = e2
    return a1 * a2, a2 * b1 + b2


def rglru_direction(u, h0, conv_w, conv_b, w_a, b_a, w_i, b_i, lam, reverse):
    left = 1 if reverse else LRU_CONV // 2
    xc = depthwise_conv(u, conv_w, conv_b, left).astype(jnp.float32)
    bsz, n, wdt = xc.shape
    xh = xc.reshape(bsz, n, LRU_HEADS, LRU_HEAD_DIM)
    r = jax.nn.sigmoid(jnp.einsum('bnhd,hde->bnhe', xh, w_a.astype(jnp.float32)).reshape(bsz, n, wdt) + b_a.astype(jnp.float32))
    i = jax.nn.sigmoid(jnp.einsum('bnhd,hde->bnhe', xh, w_i.astype(jnp.float32)).reshape(bsz, n, wdt) + b_i.astype(jnp.float32))
    a = jnp.exp(-LRU_C * r * jax.nn.softplus(-lam.astype(jnp.float32)))
    bterm = jnp.sqrt(jnp.maximum(1.0 - a * a, 0.0)) * (i * xc)
    a_cum, b_cum = lax.associative_scan(_linear_combine, (a, bterm), axis=1, reverse=reverse)
    h = a_cum * h0[:, None, :] + b_cum
    h_final = h[:, 0] if reverse else h[:, -1]
    return h, h_final


def axial_rope(v, row, col):
    half = MLA_ROPE // 2
    inv = ROPE_BASE ** (-jnp.arange(0, half, 2, dtype=jnp.float32) / half)

    def rot(z, pos):
        ang = pos[:, None] * inv[None, :]
        cos = jnp.cos(ang)[None, :, None, :]
        sin = jnp.sin(ang)[None, :, None, :]
        z1, z2 = jnp.split(z, 2, axis=-1)
        return jnp.concatenate([z1 * cos - z2 * sin, z1 * sin + z2 * cos], axis=-1)

    vf = v.astype(jnp.float32)
    out = jnp.concatenate([rot(vf[..., :half], row), rot(vf[..., half:], col)], axis=-1)
    return out.astype(v.dtype)


def mla_queries(cq, q_norm_g, w_uq, row, col):
    bsz, m, _ = cq.shape
    q = (rms_norm(cq, q_norm_g) @ w_uq).reshape(bsz, m, MLA_HEADS, MLA_NOPE + MLA_ROPE)
    if row is None:
        return q
    return jnp.concatenate([q[..., :MLA_NOPE], axial_rope(q[..., MLA_NOPE:], row, col)], axis=-1)


def mla_keys_values(ckv, krope, kv_norm_g, w_ukv, row, col):
    bsz, m, _ = ckv.shape
    kv = (rms_norm(ckv, kv_norm_g) @ w_ukv).reshape(bsz, m, MLA_HEADS, MLA_NOPE + MLA_V)
    k_nope, v = kv[..., :MLA_NOPE], kv[..., MLA_NOPE:]
    kr = krope[:, :, None, :]
    if row is not None:
        kr = axial_rope(kr, row, col)
    k = jnp.concatenate([k_nope, jnp.broadcast_to(kr, (bsz, m, MLA_HEADS, MLA_ROPE))], axis=-1)
    return k, v


def attend(q, k, v):
    s = jnp.einsum('bqhd,bkhd->bhqk', q, k, preferred_element_type=jnp.float32) * ATTN_SCALE
    p = jax.nn.softmax(s, axis=-1)
    return jnp.einsum('bhqk,bkhd->bqhd', p.astype(v.dtype), v)


def attend_query_blocks(q, k, v):
    bsz, n, h, dq = q.shape
    nb = n // Q_BLOCK
    qb = q.reshape(bsz, nb, Q_BLOCK, h, dq).transpose(1, 0, 2, 3, 4)
    o = lax.map(lambda qi: attend(qi, k, v), qb)
    return o.transpose(1, 0, 2, 3, 4).reshape(bsz, n, h * v.shape[-1])


def short_conv_mix(xin, gb, gc, w, b):
    return gb * depthwise_conv(gc * xin, w, b, SC_CONV // 2)


def mix_layer(p_ctx, p_lat, row, col, with_ctx_out, lru_conv_w, lru_conv_b, lru_w_a, lru_b_a, lru_w_i, lru_b_i,
              lru_lambda, mla_q_norm_g, mla_w_uq, mla_kv_norm_g, mla_w_ukv, sc_conv_w, sc_conv_b):
    fc, lxc, lgc, cqc, ckvc, krc, sxc, sbc, scc = split_projection(p_ctx)
    fl, lxl, lgl, cql, ckvl, krl, sxl, sbl, scl = split_projection(p_lat)
    bsz = p_lat.shape[0]

    a_lat = fourier_mix(fl)

    rec_ctx_dirs = []
    rec_lat_dirs = []
    for d, rev in enumerate((False, True)):
        prm = (lru_conv_w[d], lru_conv_b[d], lru_w_a[d], lru_b_a[d], lru_w_i[d], lru_b_i[d], lru_lambda[d])
        h0 = jnp.zeros((bsz, GROUP_W), jnp.float32)
        h_c, s_c = rglru_direction(lxc, h0, *prm, reverse=rev)
        h_l, _ = rglru_direction(lxl, s_c, *prm, reverse=rev)
        rec_ctx_dirs.append(h_c)
        rec_lat_dirs.append(h_l)
    b_lat = jax.nn.gelu(lgl) * (rec_lat_dirs[0] + rec_lat_dirs[1]).astype(lgl.dtype)

    k_ctx, v_ctx = mla_keys_values(ckvc, krc, mla_kv_norm_g, mla_w_ukv, None, None)
    k_lat, v_lat = mla_keys_values(ckvl, krl, mla_kv_norm_g, mla_w_ukv, row, col)
    q_lat = mla_queries(cql, mla_q_norm_g, mla_w_uq, row, col)
    k_all = jnp.concatenate([k_ctx, k_lat], axis=1)
    v_all = jnp.concatenate([v_ctx, v_lat], axis=1)
    c_lat = attend_query_blocks(q_lat, k_all, v_all)

    d_lat = short_conv_mix(sxl, sbl, scl, sc_conv_w, sc_conv_b)

    mix_lat = jnp.concatenate([a_lat, b_lat, c_lat, d_lat], axis=-1)
    if not with_ctx_out:
        return None, mix_lat

    a_ctx = fourier_mix(fc)
    b_ctx = jax.nn.gelu(lgc) * (rec_ctx_dirs[0] + rec_ctx_dirs[1]).astype(lgc.dtype)
    q_ctx = mla_queries(cqc, mla_q_norm_g, mla_w_uq, None, None)
    c_ctx_out = attend(q_ctx, k_ctx, v_ctx).reshape(bsz, q_ctx.shape[1], MLA_HEADS * MLA_V)
    d_ctx = short_conv_mix(sxc, sbc, scc, sc_conv_w, sc_conv_b)
    mix_ctx = jnp.concatenate([a_ctx, b_ctx, c_ctx_out, d_ctx], axis=-1)
    return mix_ctx, mix_lat


def moe_ffn(h, router_w, router_b, w_gu, b_gu, w_down, b_down):
    shp = h.shape
    t = h.reshape(-1, D_MODEL)
    n_tok = t.shape[0]
    n_asg = n_tok * TOP_K
    logits = jnp.dot(t, router_w, preferred_element_type=jnp.float32) + router_b.astype(jnp.float32)
    top_val, top_idx = lax.top_k(logits, TOP_K)
    gates = jax.nn.softmax(top_val, axis=-1).astype(h.dtype)
    flat_e = top_idx.reshape(-1)
    flat_tok = jnp.arange(n_asg, dtype=jnp.int32) // TOP_K
    order = jnp.argsort(flat_e)
    e_sorted = flat_e[order]
    counts = jnp.bincount(flat_e, length=N_EXPERTS)
    padded = (counts + MOE_BLOCK - 1) // MOE_BLOCK * MOE_BLOCK
    pad_end = jnp.cumsum(padded)
    pad_start = pad_end - padded
    grp_start = jnp.cumsum(counts) - counts
    slot = pad_start[e_sorted] + jnp.arange(n_asg, dtype=jnp.int32) - grp_start[e_sorted]
    n_blocks = (n_asg + N_EXPERTS * (MOE_BLOCK - 1) + MOE_BLOCK - 1) // MOE_BLOCK
    n_slots = n_blocks * MOE_BLOCK
    slot_tok = jnp.full((n_slots,), n_tok, jnp.int32).at[slot].set(flat_tok[order])
    slot_gate = jnp.zeros((n_slots,), h.dtype).at[slot].set(gates.reshape(-1)[order])
    block_exp = jnp.minimum(jnp.searchsorted(pad_end, jnp.arange(n_blocks, dtype=jnp.int32) * MOE_BLOCK, side='right'), N_EXPERTS - 1)
    t_pad = jnp.concatenate([t, jnp.zeros((1, D_MODEL), t.dtype)], axis=0)

    def expert_block(args):
        tok, e = args
        xb = t_pad[tok]
        gu = xb @ w_gu[e] + b_gu[e]
        g = jnp.minimum(gu[:, :D_EXPERT], SWIGLU_LIMIT)
        u = jnp.clip(gu[:, D_EXPERT:], -SWIGLU_LIMIT, SWIGLU_LIMIT)
        act = (u + 1) * (g * jax.nn.sigmoid(SWIGLU_ALPHA * g))
        return act @ w_down[e] + b_down[e]

    y = lax.map(expert_block, (slot_tok.reshape(n_blocks, MOE_BLOCK), block_exp))
    y = y.reshape(n_slots, D_MODEL) * slot_gate[:, None]
    out = jnp.zeros((n_tok + 1, D_MODEL), h.dtype).at[slot_tok].add(y)[:n_tok]
    return out.reshape(shp)


def setup_inputs(seed: int = 0) -> dict:
    key = jax.random.key(seed)
    ks = iter(jax.random.split(key, 40))
    f32 = jnp.float32
    L = DEPTH

    def nrm(shape, scale):
        return jax.random.normal(next(ks), shape, f32) * scale

    def gain(shape):
        return 1.0 + nrm(shape, 0.05)

    x = nrm((BATCH, SEQ, D_MODEL), 1.0)
    c = nrm((BATCH, D_MODEL), 1.0)
    ctx = nrm((BATCH, CTX_LEN, D_MODEL), 1.0)
    c_ctx = nrm((D_MODEL,), 1.0)
    mod_w = nrm((L, D_MODEL, 6 * D_MODEL), D_MODEL ** -0.5)
    mod_b = nrm((L, 6 * D_MODEL), 0.02)
    norm1_g = gain((L, D_MODEL))
    norm2_g = gain((L, D_MODEL))
    w_in = nrm((L, D_MODEL, P_TOTAL), D_MODEL ** -0.5)
    lru_conv_w = nrm((L, 2, LRU_CONV, GROUP_W), LRU_CONV ** -0.5)
    lru_conv_b = nrm((L, 2, GROUP_W), 0.02)
    lru_w_a = nrm((L, 2, LRU_HEADS, LRU_HEAD_DIM, LRU_HEAD_DIM), LRU_HEAD_DIM ** -0.5)
    lru_b_a = nrm((L, 2, GROUP_W), 0.1)
    lru_w_i = nrm((L, 2, LRU_HEADS, LRU_HEAD_DIM, LRU_HEAD_DIM), LRU_HEAD_DIM ** -0.5)
    lru_b_i = nrm((L, 2, GROUP_W), 0.1)
    a_c = jax.random.uniform(next(ks), (L, 2, GROUP_W), f32, 0.9, 0.999)
    a0 = a_c ** (1.0 / LRU_C)
    lru_lambda = jnp.log(a0) - jnp.log1p(-a0)
    mla_q_norm_g = gain((L, MLA_Q_LORA))
    mla_w_uq = nrm((L, MLA_Q_LORA, MLA_HEADS * (MLA_NOPE + MLA_ROPE)), MLA_Q_LORA ** -0.5)
    mla_kv_norm_g = gain((L, MLA_KV_LORA))
    mla_w_ukv = nrm((L, MLA_KV_LORA, MLA_HEADS * (MLA_NOPE + MLA_V)), MLA_KV_LORA ** -0.5)
    sc_conv_w = nrm((L, SC_CONV, GROUP_W), SC_CONV ** -0.5)
    sc_conv_b = nrm((L, GROUP_W), 0.02)
    w_out = nrm((L, D_MODEL, D_MODEL), D_MODEL ** -0.5)
    router_w = nrm((L, D_MODEL, N_EXPERTS), D_MODEL ** -0.5)
    router_b = nrm((L, N_EXPERTS), 0.01)
    exp_w_gu = nrm((L, N_EXPERTS, D_MODEL, 2 * D_EXPERT), D_MODEL ** -0.5)
    exp_b_gu = nrm((L, N_EXPERTS, 2 * D_EXPERT), 0.01)
    exp_w_down = nrm((L, N_EXPERTS, D_EXPERT, D_MODEL), D_EXPERT ** -0.5)
    exp_b_down = nrm((L, N_EXPERTS, D_MODEL), 0.01)
    final_g = gain((D_MODEL,))
    return {'x': x, 'c': c, 'ctx': ctx, 'c_ctx': c_ctx, 'mod_w': mod_w, 'mod_b': mod_b,
            'norm1_g': norm1_g, 'norm2_g': norm2_g, 'w_in': w_in,
            'lru_conv_w': lru_conv_w, 'lru_conv_b': lru_conv_b, 'lru_w_a': lru_w_a, 'lru_b_a': lru_b_a,
            'lru_w_i': lru_w_i, 'lru_b_i': lru_b_i, 'lru_lambda': lru_lambda,
            'mla_q_norm_g': mla_q_norm_g, 'mla_w_uq': mla_w_uq, 'mla_kv_norm_g': mla_kv_norm_g,
            'mla_w_ukv': mla_w_ukv, 'sc_conv_w': sc_conv_w, 'sc_conv_b': sc_conv_b, 'w_out': w_out,
            'router_w': router_w, 'router_b': router_b, 'exp_w_gu': exp_w_gu, 'exp_b_gu': exp_b_gu,
            'exp_w_down': exp_w_down, 'exp_b_down': exp_b_down, 'final_g': final_g}


def reference(x, c, ctx, c_ctx, mod_w, mod_b, norm1_g, norm2_g, w_in, lru_conv_w, lru_conv_b, lru_w_a, lru_b_a,
              lru_w_i, lru_b_i, lru_lambda, mla_q_norm_g, mla_w_uq, mla_kv_norm_g, mla_w_ukv, sc_conv_w, sc_conv_b,
              w_out, router_w, router_b, exp_w_gu, exp_b_gu, exp_w_down, exp_b_down, final_g):
    n = x.shape[1]
    grid_rows = n // GRID_W
    row = jnp.broadcast_to(jnp.arange(grid_rows, dtype=jnp.float32)[:, None], (grid_rows, GRID_W)).reshape(-1)
    col = jnp.broadcast_to(jnp.arange(GRID_W, dtype=jnp.float32)[None, :], (grid_rows, GRID_W)).reshape(-1)
    n_ctx = ctx.shape[1]

    h_lat = x
    h_ctx = ctx
    for l in range(DEPTH):
        with_ctx_out = l < DEPTH - 1
        mod_lat = (jax.nn.silu(c) @ mod_w[l] + mod_b[l])[:, None, :]
        mod_ctx = (jax.nn.silu(c_ctx) @ mod_w[l] + mod_b[l])[None, None, :]
        sh1_l, sc1_l, g1_l, sh2_l, sc2_l, g2_l = jnp.split(mod_lat, 6, axis=-1)
        sh1_c, sc1_c, g1_c, sh2_c, sc2_c, g2_c = jnp.split(mod_ctx, 6, axis=-1)

        a_lat = modulate(rms_norm(h_lat, norm1_g[l]), sh1_l, sc1_l)
        a_ctx = modulate(rms_norm(h_ctx, norm1_g[l]), sh1_c, sc1_c)
        p_lat = a_lat @ w_in[l]
        p_ctx = a_ctx @ w_in[l]
        mix_ctx, mix_lat = mix_layer(p_ctx, p_lat, row, col, with_ctx_out,
                                     lru_conv_w[l], lru_conv_b[l], lru_w_a[l], lru_b_a[l], lru_w_i[l], lru_b_i[l],
                                     lru_lambda[l], mla_q_norm_g[l], mla_w_uq[l], mla_kv_norm_g[l], mla_w_ukv[l],
                                     sc_conv_w[l], sc_conv_b[l])
        h_lat = h_lat + g1_l * (mix_lat @ w_out[l])

        f_lat = modulate(rms_norm(h_lat, norm2_g[l]), sh2_l, sc2_l)
        moe_args = (router_w[l], router_b[l], exp_w_gu[l], exp_b_gu[l], exp_w_down[l], exp_b_down[l])
        if with_ctx_out:
            h_ctx = h_ctx + g1_c * (mix_ctx @ w_out[l])
            f_ctx = modulate(rms_norm(h_ctx, norm2_g[l]), sh2_c, sc2_c)
            y_all = moe_ffn(jnp.concatenate([f_ctx, f_lat], axis=1), *moe_args)
            h_ctx = h_ctx + g2_c * y_all[:, :n_ctx]
            h_lat = h_lat + g2_l * y_all[:, n_ctx:]
        else:
            h_lat = h_lat + g2_l * moe_ffn(f_lat, *moe_args)

    return rms_norm(h_lat, final_g)
```

```python
import numpy as np
import ml_dtypes
from contextlib import ExitStack
import concourse.bass as bass
import concourse.mybir as mybir
from concourse.bass_utils import run_bass_kernel_spmd

F32 = mybir.dt.float32
BF16 = mybir.dt.bfloat16
AF = mybir.ActivationFunctionType
ALU = mybir.AluOpType
AX = mybir.AxisListType

DEBUG = {}

D = 1024
NTOK = 4352
NB = 34
NOUT = 2304
NOB = 18
OWN0 = 2304
EPS = 1e-6
NEXP = 32
ATTN_SCALE = 96.0 ** -0.5
C_FN, C_LX, C_LG, C_CQ, C_KV, C_KR, C_SX, C_SB, C_SC = 0, 256, 512, 768, 1024, 1152, 1184, 1440, 1696


def _isz(dt):
    return np.dtype(mybir.dt.np(dt)).itemsize


class Fw:
    KDMA = 8

    def __init__(self, nc, stack):
        self.nc = nc
        self.stack = stack
        self.E = {'pe': nc.tensor, 'dve': nc.vector, 'act': nc.scalar, 'pool': nc.gpsimd, 'sp': nc.sync}
        self.sem = {}
        self.tick = {}
        self.seen = {n: {} for n in self.E}
        for n in self.E:
            self.sem[n] = stack.enter_context(nc.semaphore("clk_" + n))
            self.tick[n] = 0
        self.dq = {}
        self.recs = {}
        self.readonly = set()
        self.n_wait = 0
        self.n_ins = 0
        self.out_tokens = []
        self.nps = 0

    def _region(self, ap):
        t = ap.tensor.name
        if t in self.readonly:
            return None
        dims = ap.ap
        off = int(ap.offset)
        isz = _isz(ap.dtype)
        sp = str(ap.space)
        if sp in ('SB', 'PSUM') or 'SB' in sp or 'PSUM' in sp:
            R = dims[0][0]
            if R == 0:
                R = 1 << 30
            p0 = off // R
            c0 = off % R
            p1 = p0 + dims[0][1] - 1
            lo = c0 + sum(min(0, s * (n - 1)) for s, n in dims[1:])
            hi = c0 + sum(max(0, s * (n - 1)) for s, n in dims[1:])
            return (t, p0, p1, lo * isz, hi * isz + isz - 1)
        lo = off + sum(min(0, s * (n - 1)) for s, n in dims)
        hi = off + sum(max(0, s * (n - 1)) for s, n in dims)
        return (t, 0, 0, lo * isz, hi * isz + isz - 1)

    @staticmethod
    def _ov(a, b):
        return a[1] <= b[2] and b[1] <= a[2] and a[3] <= b[4] and b[3] <= a[4]

    def _need(self, eng, tok):
        key, val = tok
        if self.seen[eng].get(key, 0) >= val:
            return
        sem = self.sem[key] if key in self.sem else self.dq[key[0]]['sems'][key[1]]
        self.E[eng].wait_ge(sem, val)
        self.n_wait += 1
        self.seen[eng][key] = val

    def _deps(self, eng, rr, ww, exempt=True):
        ex = eng if (exempt and eng == 'pe') else None
        for reg in rr:
            for rec in self.recs.get(reg[0], ()):
                if rec[1] is not None and self._ov(rec[0], reg):
                    self._need(eng, rec[1])
        for reg in ww:
            for rec in self.recs.get(reg[0], ()):
                if self._ov(rec[0], reg):
                    if rec[1] is not None and rec[1][0] != ex:
                        self._need(eng, rec[1])
                    for k, v in rec[2].items():
                        if k != ex:
                            self._need(eng, (k, v))

    def _commit(self, tok, rr, ww):
        for reg in rr:
            lst = self.recs.setdefault(reg[0], [])
            for rec in lst:
                if rec[0] == reg:
                    rec[2][tok[0]] = max(rec[2].get(tok[0], 0), tok[1])
                    break
            else:
                lst.append([reg, None, {tok[0]: tok[1]}])
        for reg in ww:
            lst = self.recs.setdefault(reg[0], [])
            lst[:] = [r for r in lst if not (reg[1] <= r[0][1] and r[0][2] <= reg[2] and reg[3] <= r[0][3] and r[0][4] <= reg[4])]
            lst.append([reg, tok, {}])

    def _regs(self, aps):
        out = []
        for a in aps:
            if a is None or isinstance(a, (int, float)):
                continue
            r = self._region(a)
            if r is not None:
                out.append(r)
        return out

    def op(self, eng, fn, ins=(), outs=()):
        rr = self._regs(ins)
        ww = self._regs(outs)
        self._deps(eng, rr, ww)
        ins_ = fn(self.E[eng])
        self.tick[eng] += 1
        ins_.then_inc(self.sem[eng], 1)
        self.n_ins += 1
        tok = (eng, self.tick[eng])
        self._commit(tok, rr, ww)
        return tok

    def dma(self, q, out, in_, is_output=False, **kw):
        d = self.dq.get(q)
        if d is None:
            d = {'sems': [self.stack.enter_context(self.nc.semaphore("dq_%s_%d" % (q, i))) for i in range(self.KDMA)], 'n': 0}
            self.dq[q] = d
        i = d['n']
        slot = i % self.KDMA
        if i >= self.KDMA:
            self._need(q, ((q, slot), 16 * (i // self.KDMA)))
        rr = self._regs([in_])
        ww = self._regs([out])
        self._deps(q, rr, ww, exempt=False)
        ins_ = self.E[q].dma_start(out=out, in_=in_, **kw)
        ins_.then_inc(d['sems'][slot], 16)
        d['n'] += 1
        self.n_ins += 1
        tok = ((q, slot), 16 * (i // self.KDMA + 1))
        self._commit(tok, rr, ww)
        if is_output:
            self.out_tokens.append(tok)
        return tok

    def barrier(self):
        toks = [(n, self.tick[n]) for n in self.E if self.tick[n] > 0]
        for q, d in self.dq.items():
            for slot in range(self.KDMA):
                cnt = (d['n'] - slot + self.KDMA - 1) // self.KDMA
                if cnt > 0:
                    toks.append(((q, slot), 16 * cnt))
        for e in self.E:
            for t in toks:
                if t[0] != e:
                    self._need(e, t)
        self.recs = {}

    def finish(self):
        for tok in self.out_tokens:
            self._need('sp', tok)


class Emit:
    def __init__(self, nc, fw, io):
        self.nc = nc
        self.fw = fw
        self.io = io
        self.tsuf = ''
        self.msks = {}

    def tab(self, name):
        return self.io[name + self.tsuf]

    @property
    def msk(self):
        return self.msks[self.tsuf]

    def tt(self, out, in0, in1, op, eng='dve'):
        self.fw.op(eng, lambda e: e.tensor_tensor(out=out, in0=in0, in1=in1, op=op), ins=[in0, in1], outs=[out])

    def ts(self, out, in0, s1, s2, op0, op1=None, eng='dve'):
        if op1 is None:
            self.fw.op(eng, lambda e: e.tensor_scalar(out=out, in0=in0, scalar1=s1, scalar2=None, op0=op0), ins=[in0, s1], outs=[out])
        else:
            self.fw.op(eng, lambda e: e.tensor_scalar(out=out, in0=in0, scalar1=s1, scalar2=s2, op0=op0, op1=op1), ins=[in0, s1, s2], outs=[out])

    def stt(self, out, in0, scalar, in1, op0, op1):
        self.fw.op('dve', lambda e: e.scalar_tensor_tensor(out=out, in0=in0, scalar=scalar, in1=in1, op0=op0, op1=op1),
                   ins=[in0, scalar, in1], outs=[out])

    def act(self, out, in_, func, bias=None, scale=None, accum=None):
        kw = {}
        if bias is not None:
            kw['bias'] = bias
        if scale is not None:
            kw['scale'] = scale
        if accum is not None:
            kw['accum_out'] = accum
        self.fw.op('act', lambda e: e.activation(out=out, in_=in_, func=func, **kw), ins=[in_, bias, scale], outs=[out, accum])

    def cp(self, eng, out, in_):
        if eng == 'act':
            self.fw.op('act', lambda e: e.copy(out=out, in_=in_), ins=[in_], outs=[out])
        else:
            self.fw.op(eng, lambda e: e.tensor_copy(out=out, in_=in_), ins=[in_], outs=[out])

    def memset(self, eng, ap, v):
        self.fw.op(eng, lambda e: e.memset(ap, v), outs=[ap])

    def mm(self, out, lhsT, rhs, start, stop):
        self.fw.op('pe', lambda e: e.matmul(out, lhsT, rhs, start=start, stop=stop), ins=[lhsT, rhs], outs=[out])

    def tr(self, out, in_, ident):
        self.fw.op('pe', lambda e: e.transpose(out, in_, ident), ins=[in_, ident], outs=[out])

    def recip(self, out, in_):
        self.fw.op('dve', lambda e: e.reciprocal(out=out, in_=in_), ins=[in_], outs=[out])

    def ps(self):
        i = self.fw.nps % 8
        self.fw.nps += 1
        return self.P[i]

    def sb(self, st, name, shape, dt):
        self.fw.nsb = getattr(self.fw, 'nsb', 0) + 1
        return st.enter_context(self.nc.sbuf_tensor("%s_s%d" % (name, self.fw.nsb), list(shape), dt))

    def setup(self, st):
        nc = self.nc
        self.P = [st.enter_context(nc.psum_tensor("ps%d" % i, [128, 512], F32)) for i in range(8)]
        self.ident = self.sb(st, "ident", [128, 128], F32)
        self.onesf = self.sb(st, "onesf", [128, 128], F32)
        self.prot = self.sb(st, "prot", [96, 96], F32)
        self.fw.dma('sp', self.ident[:], self.io['ident'][:, :])
        for suf in ('', '_B'):
            if ('msk' + suf) in self.io:
                self.msks[suf] = self.sb(st, "msk" + suf, [128, 8], F32)
                self.fw.dma('sp', self.msks[suf][:], self.io['msk' + suf][:, :])
        self.fw.dma('sp', self.prot[:], self.io['prot'][:, :])
        self.memset('pool', self.onesf[:], 1.0)
        self.aT_d = nc.dram_tensor("aT_d", [128, 8, NTOK], BF16, kind="Internal").ap()
        self.mod_d = nc.dram_tensor("mod_d", [2, 6 * D], F32, kind="Internal").ap()
        self.hmid_d = nc.dram_tensor("hmid_d", [NOUT, D], F32, kind="Internal").ap()

    def colload(self, st, name, src_rows, ncols):
        R = sum(a.shape[0] for a in src_rows)
        nch = ncols // 128
        dst = self.sb(st, name, [128, nch, R], F32)
        with ExitStack() as s2:
            stg = self.sb(s2, name + "_stg", [R, ncols], F32)
            r0 = 0
            for a in src_rows:
                self.fw.dma('sp', stg[r0:r0 + a.shape[0], :], a)
                r0 += a.shape[0]
            for c in range(nch):
                p = self.ps()
                self.tr(p[:, 0:R], stg[0:R, c * 128:(c + 1) * 128], self.ident[0:R, 0:R])
                self.cp('dve', dst[:, c, :], p[:, 0:R])
            self.fw.barrier()
        return dst

    def bcload(self, tile_ap, row_ap):
        self.fw.dma('sp', tile_ap, row_ap.partition_broadcast(128))

    def adaln(self, L):
        io = self.io
        with ExitStack() as st:
            cv = self.sb(st, "cv", [2, D], F32)
            sT = self.sb(st, "sT", [128, 8, 2], F32)
            mb = self.sb(st, "mb", [2, 6 * D], F32)
            mrow = self.sb(st, "mrow", [2, 6 * D], F32)
            wst = [self.sb(st, "modw%d" % i, [128, 8, 512], F32) for i in range(2)]
            self.fw.dma('sp', cv[:], io['cvec'][:, :])
            self.fw.dma('sp', mb[0:1, :], io['mod_b'][L:L + 1, :])
            self.fw.dma('sp', mb[1:2, :], io['mod_b'][L:L + 1, :])
            self.act(cv[:], cv[:], AF.Silu)
            for kc in range(8):
                p = self.ps()
                self.tr(p[:, 0:2], cv[0:2, kc * 128:(kc + 1) * 128], self.ident[0:2, 0:2])
                self.cp('dve', sT[:, kc, :], p[:, 0:2])
            mw = io['mod_w'][L].rearrange("(kc p) f -> p kc f", p=128)
            for cg in range(12):
                w = wst[cg % 2]
                self.fw.dma('sp' if cg % 2 == 0 else 'act', w[:], mw[:, :, cg * 512:(cg + 1) * 512])
                p = self.ps()
                for kc in range(8):
                    self.mm(p[0:2, :], sT[:, kc, :], w[:, kc, :], kc == 0, kc == 7)
                self.tt(mrow[:, cg * 512:(cg + 1) * 512], p[0:2, :], mb[:, cg * 512:(cg + 1) * 512], ALU.add)
            self.fw.dma('sp', self.mod_d[:, :], mrow[:])
            self.fw.barrier()

    def pass0(self, L, hsrc):
        io = self.io
        with ExitStack() as st:
            A = [self.sb(st, "A1_%d" % j, [128, D], F32) for j in range(2)]
            S = [self.sb(st, "S1_%d" % j, [128, D], F32) for j in range(2)]
            gb = self.sb(st, "g1n", [128, D], F32)
            self.bcload(gb[:], io['norm1_g'][L:L + 1, :])
            for j in range(2):
                self.bcload(S[j][:], self.mod_d[j:j + 1, 0:D])
                self.bcload(A[j][:], self.mod_d[j:j + 1, D:2 * D])
                self.ts(A[j][:], A[j][:], 1.0, None, ALU.add)
                self.tt(A[j][:], A[j][:], gb[:], ALU.mult)
            hb = [self.sb(st, "hb%d" % i, [128, D], F32) for i in range(2)]
            tb = [self.sb(st, "tb%d" % i, [128, D], F32) for i in range(2)]
            junk = self.sb(st, "junk", [128, D], F32)
            ss = [self.sb(st, "ss%d" % i, [128, 1], F32) for i in range(2)]
            ab = [self.sb(st, "ab%d" % i, [128, 8, 128], BF16) for i in range(2)]
            for blk in range(NB):
                j = 0 if blk >= 2 else 1
                h = hb[blk % 2]
                t = tb[blk % 2]
                s_ = ss[blk % 2]
                a_ = ab[blk % 2]
                self.fw.dma('sp', h[:], hsrc[blk * 128:(blk + 1) * 128, :])
                self.act(junk[:], h[:], AF.Square, accum=s_[:])
                self.act(s_[:], s_[:], AF.Sqrt, bias=EPS, scale=1.0 / D)
                self.recip(s_[:], s_[:])
                self.stt(t[:], h[:], s_[:, 0:1], A[j][:], ALU.mult, ALU.mult)
                self.tt(t[:], t[:], S[j][:], ALU.add, eng='pool')
                for half in range(2):
                    p = self.ps()
                    for q in range(4):
                        kc = half * 4 + q
                        self.tr(p[:, q * 128:(q + 1) * 128], t[:, kc * 128:(kc + 1) * 128], self.ident[:])
                    self.cp('act', a_[:, half * 4:(half + 1) * 4, :], p[:, :].rearrange("p (q t) -> p q t", q=4))
                self.fw.dma('pool', self.aT_d[:, :, blk * 128:(blk + 1) * 128], a_[:])
            self.fw.barrier()

    GROUPS = [(0, 256)] + [(256 + 512 * i, 512) for i in range(8)]
    OGROUPS = [(0, 256, 0)] + [(OWN0 + 512 * i, 512, 256 + 512 * i) for i in range(4)]

    def load_win(self, st, name, c0, ncols, L):
        w = self.sb(st, name, [128, 8, ncols], BF16)
        src = self.io['w_in'][L].rearrange("(kc p) f -> p kc f", p=128)
        self.fw.dma('pool', w[:], src[:, :, c0:c0 + ncols])
        return w

    def proj(self, p_ap, w, c0, m, aT, n):
        for kc in range(8):
            self.mm(p_ap, w[:, kc, c0:c0 + m], aT[:, kc, 0:n], kc == 0, kc == 7)

    def pass_fnet(self, L, mixT):
        io = self.io
        with ExitStack() as st:
            wf = self.load_win(st, "wf", C_FN, 256, L)
            cs = self.sb(st, "cs64", [128, 2, 512], BF16)
            self.fw.dma('sp', cs[:], io['cs64'].rearrange("(cc p) f -> p cc f", p=128))
            V = self.sb(st, "Vf", [128, NB, 512], BF16)
            aTg = [self.sb(st, "aTg%d" % i, [128, 8, 512], BF16) for i in range(2)]
            UT = [self.sb(st, "UT%d" % i, [128, 2, 512], BF16) for i in range(2)]
            for gi, (t0, n) in enumerate(self.GROUPS):
                a = aTg[gi % 2]
                u = UT[gi % 2]
                self.fw.dma('sp', a[:, :, 0:n], self.aT_d[:, :, t0:t0 + n])
                for cc in range(2):
                    p = self.ps()
                    self.proj(p[:, 0:n], wf, cc * 128, 128, a, n)
                    self.cp('act', u[:, cc, 0:n], p[:, 0:n])
                for b in range(n // 128):
                    p = self.ps()
                    for cc in range(2):
                        self.mm(p[:, :], u[:, cc, b * 128:(b + 1) * 128], cs[:, cc, :], cc == 0, cc == 1)
                    self.cp('dve', V[:, t0 // 128 + b, :], p[:, :])
            d2 = self.sb(st, "d256", [128, 2, 2, 256], BF16)
            self.fw.dma('sp', d2[:], io['dft256'].rearrange("t (nb p) k -> p t nb k", p=128))
            for jc in range(2):
                p = self.ps()
                i = 0
                for nb in range(2):
                    for tbl in range(2):
                        self.mm(p[:, 0:256], V[:, nb, tbl * 256 + jc * 128: tbl * 256 + (jc + 1) * 128], d2[:, tbl, nb, :], i == 0, i == 3)
                        i += 1
                self.cp('act', mixT[:, jc, 0:256], p[:, 0:256])
            dt_ = [[self.sb(st, "dft%d_%d" % (t, i), [128, 8, 512], BF16) for i in range(2)] for t in range(2)]
            cnt = 0
            for kg in range(4):
                pj = [self.ps(), self.ps()]
                for nch in range(4):
                    tl = [dt_[0][cnt % 2], dt_[1][cnt % 2]]
                    cnt += 1
                    self.fw.dma('sp', tl[0][:], self.tab('dftc')[kg, :, nch * 8:(nch + 1) * 8, :])
                    self.fw.dma('act', tl[1][:], self.tab('dfts')[kg, :, nch * 8:(nch + 1) * 8, :])
                    for nb8 in range(8):
                        nb = nch * 8 + nb8
                        for tbl in range(2):
                            for jc in range(2):
                                first = (nb == 0 and tbl == 0)
                                last = (nb == 31 and tbl == 1)
                                self.mm(pj[jc][:, :], V[:, 2 + nb, tbl * 256 + jc * 128: tbl * 256 + (jc + 1) * 128], tl[tbl][:, nb8, :], first, last)
                for jc in range(2):
                    self.cp('act' if jc == 0 else 'dve', mixT[:, jc, 256 + kg * 512:256 + (kg + 1) * 512], pj[jc][:, :])
            self.fw.barrier()

    def gelu_tanh(self, out, x, tmp):
        self.tt(tmp, x, x, ALU.mult)
        self.ts(tmp, tmp, 0.044715, 1.0, ALU.mult, ALU.add)
        self.tt(tmp, tmp, x, ALU.mult)
        self.act(tmp, tmp, AF.Sigmoid, scale=2.0 * 0.7978845608028654)
        self.tt(out, tmp, x, ALU.mult)

    def pass_lru(self, L, mixT, P256):
        io = self.io
        NE = 4364
        CT0, PA0, OW0 = 2, 262, 2314
        with ExitStack() as st:
            X = self.sb(st, "lruX", [128, 2, NE], F32)
            lg = self.sb(st, "lgate", [128, 2, NOUT], BF16)
            rec = self.sb(st, "rec", [128, 2, NOUT], F32)
            Wg = self.sb(st, "Wg", [128, 2, 2, 2, 128], BF16)
            asc = self.sb(st, "asc", [128, 2, 2], F32)
            s2 = ExitStack()
            wlx = self.load_win(s2, "wlx", C_LX, 512, L)
            aTg = [self.sb(s2, "aTg%d" % i, [128, 8, 512], BF16) for i in range(2)]
            gx = self.sb(s2, "gx", [128, 512], F32)
            gt = self.sb(s2, "gt", [128, 512], F32)
            self.memset('pool', Wg[:], 0.0)
            for gi_, nm in enumerate(('lru_w_a', 'lru_w_i')):
                for d in range(2):
                    for hh in range(4):
                        cc, o = hh // 2, (hh % 2) * 64
                        self.fw.dma('pool', Wg[o:o + 64, gi_, d, cc, o:o + 64], io[nm][L, d, hh])
            for cc in range(2):
                self.memset('pool', X[:, cc, 0:2], 0.0)
                self.memset('pool', X[:, cc, 258:262], 0.0)
                self.memset('pool', X[:, cc, 2310:2314], 0.0)
                self.memset('pool', X[:, cc, 4362:4364], 0.0)
            ext0 = {0: CT0}
            for i in range(4):
                ext0[256 + 512 * i] = PA0 + 512 * i
                ext0[OWN0 + 512 * i] = OW0 + 512 * i
            ostart = {t0: o0 for (t0, n, o0) in self.OGROUPS}
            for gi, (t0, n) in enumerate(self.GROUPS):
                a = aTg[gi % 2]
                self.fw.dma('sp', a[:, :, 0:n], self.aT_d[:, :, t0:t0 + n])
                for cc in range(2):
                    p = self.ps()
                    self.proj(p[:, 0:n], wlx, cc * 128, 128, a, n)
                    self.cp('act', X[:, cc, ext0[t0]:ext0[t0] + n], p[:, 0:n])
                if t0 in ostart:
                    o0 = ostart[t0]
                    for cc in range(2):
                        p = self.ps()
                        self.proj(p[:, 0:n], wlx, 256 + cc * 128, 128, a, n)
                        self.cp('act', gx[:, 0:n], p[:, 0:n])
                        self.gelu_tanh(lg[:, cc, o0:o0 + n], gx[:, 0:n], gt[:, 0:n])
            self.fw.barrier()
            s2.close()
            m0, m1 = self.msk[:, 0:1], self.msk[:, 1:2]
            for cc in range(2):
                self.ts(X[:, cc, PA0 - 2:PA0], X[:, cc, OW0 + 2046:OW0 + 2048], m0, None, ALU.mult)
                self.ts(X[:, cc, PA0 + 2048:PA0 + 2050], X[:, cc, OW0:OW0 + 2], m1, None, ALU.mult)
                self.ts(X[:, cc, OW0 - 2:OW0], X[:, cc, PA0 + 2046:PA0 + 2048], m1, None, ALU.mult)
                self.ts(X[:, cc, OW0 + 2048:OW0 + 2050], X[:, cc, PA0:PA0 + 2], m0, None, ALU.mult)
            for cc in range(2):
                self.act(asc[:, cc, :], P256[:, cc, 14:16], AF.Exp, scale=-1.0)
                self.act(asc[:, cc, :], asc[:, cc, :], AF.Ln, bias=1.0)
                self.ts(asc[:, cc, :], asc[:, cc, :], -8.0, None, ALU.mult)
            xc = self.sb(st, "xc", [128, NE], F32)
            ra = self.sb(st, "ra", [128, NE], F32)
            ib = self.sb(st, "ib", [128, NE], F32)
            hs = self.sb(st, "hs", [128, NE], F32)
            xcb = self.sb(st, "xcb", [128, NE], BF16)
            E0, E1 = 2, 4362
            chunks = [(E0 + 512 * i, min(512, E1 - (E0 + 512 * i))) for i in range((E1 - E0 + 511) // 512)]
            for d in range(2):
                left = 2 if d == 0 else 1
                md, omd = self.msk[:, 2 + d:3 + d], self.msk[:, 4 + d:5 + d]
                for cc in range(2):
                    w = lambda k: P256[:, cc, d * 4 + k:d * 4 + k + 1]
                    self.ts(xc[:, E0:E1], X[:, cc, E0 - left:E1 - left], w(0), P256[:, cc, 8 + d:9 + d], ALU.mult, ALU.add)
                    for k in range(1, 4):
                        self.stt(xc[:, E0:E1], X[:, cc, E0 - left + k:E1 - left + k], w(k), xc[:, E0:E1], ALU.mult, ALU.add)
                    self.cp('pool', xcb[:, E0:E1], xc[:, E0:E1])
                    for (c0, n) in chunks:
                        p = self.ps()
                        self.mm(p[:, 0:n], Wg[:, 0, d, cc, :], xcb[:, c0:c0 + n], True, True)
                        self.act(ra[:, c0:c0 + n], p[:, 0:n], AF.Sigmoid, bias=P256[:, cc, 10 + d:11 + d])
                        p2 = self.ps()
                        self.mm(p2[:, 0:n], Wg[:, 1, d, cc, :], xcb[:, c0:c0 + n], True, True)
                        self.act(ib[:, c0:c0 + n], p2[:, 0:n], AF.Sigmoid, bias=P256[:, cc, 12 + d:13 + d])
                    self.act(ra[:, E0:E1], ra[:, E0:E1], AF.Exp, scale=asc[:, cc, d:d + 1])
                    self.tt(hs[:, E0:E1], ra[:, E0:E1], ra[:, E0:E1], ALU.mult)
                    self.ts(hs[:, E0:E1], hs[:, E0:E1], 1.0, -1.0, ALU.min, ALU.mult)
                    self.act(hs[:, E0:E1], hs[:, E0:E1], AF.Sqrt, bias=1.0)
                    self.tt(ib[:, E0:E1], ib[:, E0:E1], xc[:, E0:E1], ALU.mult, eng='pool')
                    self.tt(ib[:, E0:E1], ib[:, E0:E1], hs[:, E0:E1], ALU.mult)
                    self.ts(ra[:, PA0:PA0 + 2048], ra[:, PA0:PA0 + 2048], md, omd, ALU.mult, ALU.add)
                    self.ts(ib[:, PA0:PA0 + 2048], ib[:, PA0:PA0 + 2048], md, None, ALU.mult)
                    segs = [(CT0, 256), (PA0, 2048), (OW0, 2048)]
                    init = 0.0
                    for (s0, n) in segs:
                        if d == 0:
                            sl = slice(s0, s0 + n)
                            last = slice(s0 + n - 1, s0 + n)
                        else:
                            sl = slice(s0 + n - 1, s0 - 1, -1)
                            last = slice(s0, s0 + 1)
                        self.fw.op('dve', lambda e: e.tensor_tensor_scan(out=hs[:, sl], data0=ra[:, sl], data1=ib[:, sl], initial=init,
                                                                         op0=ALU.mult, op1=ALU.add),
                                   ins=[ra[:, sl], ib[:, sl], init], outs=[hs[:, sl]])
                        init = hs[:, last]
                    if d == 0:
                        self.cp('pool', rec[:, cc, 0:256], hs[:, CT0:CT0 + 256])
                        self.cp('pool', rec[:, cc, 256:NOUT], hs[:, OW0:OW0 + 2048])
                    else:
                        self.tt(rec[:, cc, 0:256], rec[:, cc, 0:256], hs[:, CT0:CT0 + 256], ALU.add, eng='pool')
                        self.tt(rec[:, cc, 256:NOUT], rec[:, cc, 256:NOUT], hs[:, OW0:OW0 + 2048], ALU.add, eng='pool')
            for cc in range(2):
                self.tt(mixT[:, 2 + cc, :], lg[:, cc, :], rec[:, cc, :], ALU.mult)
            self.fw.barrier()

    def pass_attn(self, L, mixT, P256):
        io = self.io
        with ExitStack() as st:
            wcq = self.load_win(st, "wcq", C_CQ, 256, L)
            wkv = self.load_win(st, "wkv", C_KV, 160, L)
            wuq = self.sb(st, "wuq", [128, 2, 384], BF16)
            self.fw.dma('pool', wuq[:], io['mla_w_uq'][L].rearrange("(ic p) f -> p ic f", p=128))
            wukv = self.sb(st, "wukv", [128, 4, 128], BF16)
            self.fw.dma('pool', wukv[:], io['mla_w_ukv'][L].rearrange("p (h f) -> p h f", h=4))
            kvg = self.colload(st, "kvg", [io['mla_kv_norm_g'][L:L + 1, :]], 128)
            kT = self.sb(st, "kT", [96, 4, NTOK], BF16)
            Vat = self.sb(st, "Vat", [128, NB, 256], BF16)
            qT = self.sb(st, "qT", [96, 4, NOUT], BF16)
            ostart = {t0: o0 for (t0, n, o0) in self.OGROUPS}
            with ExitStack() as s2:
                aTg = [self.sb(s2, "aTg%d" % i, [128, 8, 512], BF16) for i in range(2)]
                ckv = self.sb(s2, "ckv", [128, 512], F32)
                sq = self.sb(s2, "sq", [128, 2, 512], F32)
                rstd = self.sb(s2, "rstd", [128, 512], F32)
                ckvn = self.sb(s2, "ckvn", [128, 512], BF16)
                kr = self.sb(s2, "kr", [96, 512], F32)
                rkc = self.sb(s2, "rkc", [96, 512], F32)
                rks = self.sb(s2, "rks", [96, 512], F32)
                t1 = self.sb(s2, "t1", [96, 512], F32)
                t2 = self.sb(s2, "t2", [96, 512], F32)
                cq = self.sb(s2, "cq", [128, 2, 512], F32)
                cqn = self.sb(s2, "cqn", [128, 2, 512], BF16)
                qs = self.sb(s2, "qs", [96, 512], F32)
                for gi, (t0, n) in enumerate(self.GROUPS):
                    a = aTg[gi % 2]
                    self.fw.dma('sp', a[:, :, 0:n], self.aT_d[:, :, t0:t0 + n])
                    self.fw.dma('act', rkc[:, 0:n], self.tab('ropek_c')[:, t0:t0 + n])
                    self.fw.dma('act', rks[:, 0:n], self.tab('ropek_s')[:, t0:t0 + n])
                    p = self.ps()
                    self.proj(p[:, 0:n], wkv, 0, 128, a, n)
                    self.cp('act', ckv[:, 0:n], p[:, 0:n])
                    self.act(sq[:, 0, 0:n], ckv[:, 0:n], AF.Square)
                    p = self.ps()
                    self.mm(p[:, 0:n], self.onesf[:, :], sq[:, 0, 0:n], True, True)
                    self.act(rstd[:, 0:n], p[:, 0:n], AF.Sqrt, bias=EPS, scale=1.0 / 128)
                    self.recip(rstd[:, 0:n], rstd[:, 0:n])
                    self.stt(ckvn[:, 0:n], ckv[:, 0:n], kvg[:, 0, 0:1], rstd[:, 0:n], ALU.mult, ALU.mult)
                    for h in range(4):
                        p = self.ps()
                        self.mm(p[0:64, 0:n], wukv[:, h, 0:64], ckvn[:, 0:n], True, True)
                        self.cp('act' if h % 2 == 0 else 'dve', kT[0:64, h, t0:t0 + n], p[0:64, 0:n])
                    for b in range(n // 128):
                        p = self.ps()
                        self.mm(p[:, 0:256].rearrange("p (h f) -> p h f", h=4), ckvn[:, b * 128:(b + 1) * 128], wukv[:, :, 64:128], True, True)
                        self.cp('dve', Vat[:, t0 // 128 + b, :], p[:, 0:256])
                    p = self.ps()
                    self.proj(p[0:96, 0:n], wkv, 64, 96, a, n)
                    self.cp('act', kr[:, 0:n], p[0:96, 0:n])
                    p = self.ps()
                    self.mm(p[0:96, 0:n], self.prot[:, :], kr[:, 0:n], True, True)
                    self.tt(t1[64:96, 0:n], kr[64:96, 0:n], rkc[64:96, 0:n], ALU.mult)
                    self.tt(t2[64:96, 0:n], p[64:96, 0:n], rks[64:96, 0:n], ALU.mult)
                    for h in range(4):
                        self.tt(kT[64:96, h, t0:t0 + n], t1[64:96, 0:n], t2[64:96, 0:n], ALU.add, eng='pool' if h % 2 else 'dve')
                    if t0 not in ostart:
                        continue
                    o0 = ostart[t0]
                    rqc = rkc
                    rqs = rks
                    self.fw.dma('act', rqc[:, 0:n], self.tab('ropeq_c')[:, o0:o0 + n])
                    self.fw.dma('act', rqs[:, 0:n], self.tab('ropeq_s')[:, o0:o0 + n])
                    for ic in range(2):
                        p = self.ps()
                        self.proj(p[:, 0:n], wcq, ic * 128, 128, a, n)
                        self.cp('act', cq[:, ic, 0:n], p[:, 0:n])
                        self.act(sq[:, ic, 0:n], cq[:, ic, 0:n], AF.Square)
                    p = self.ps()
                    for ic in range(2):
                        self.mm(p[:, 0:n], self.onesf[:, :], sq[:, ic, 0:n], ic == 0, ic == 1)
                    self.act(rstd[:, 0:n], p[:, 0:n], AF.Sqrt, bias=EPS, scale=1.0 / 256)
                    self.recip(rstd[:, 0:n], rstd[:, 0:n])
                    for ic in range(2):
                        self.stt(cqn[:, ic, 0:n], cq[:, ic, 0:n], P256[:, ic, 20:21], rstd[:, 0:n], ALU.mult, ALU.mult)
                    for h in range(4):
                        p = self.ps()
                        for ic in range(2):
                            self.mm(p[0:96, 0:n], wuq[:, ic, h * 96:(h + 1) * 96], cqn[:, ic, 0:n], ic == 0, ic == 1)
                        self.cp('act', qs[:, 0:n], p[0:96, 0:n])
                        p2 = self.ps()
                        self.mm(p2[0:96, 0:n], self.prot[:, :], qs[:, 0:n], True, True)
                        self.tt(t1[:, 0:n], qs[:, 0:n], rqc[:, 0:n], ALU.mult)
                        self.tt(t2[:, 0:n], p2[0:96, 0:n], rqs[:, 0:n], ALU.mult)
                        self.tt(qT[:, h, o0:o0 + n], t1[:, 0:n], t2[:, 0:n], ALU.add, eng='pool')
                self.fw.barrier()
            with ExitStack() as s3:
                ssb = self.sb(s3, "ssb", [128, NTOK], F32)
                pe_ = self.sb(s3, "pexp", [128, NTOK], F32)
                pT = self.sb(s3, "pT", [128, NB, 128], BF16)
                osb = self.sb(s3, "osb", [128, 256], F32)
                st4 = self.sb(s3, "st4", [128, 4], F32)
                for ob in range(NOB):
                    nk = 256 if ob < 2 else NTOK
                    for h in range(4):
                        k0 = 0
                        while k0 < nk:
                            n = min(512, nk - k0)
                            p = self.ps()
                            self.mm(p[:, 0:n], qT[:, h, ob * 128:(ob + 1) * 128], kT[:, h, k0:k0 + n], True, True)
                            self.cp('act', ssb[:, k0:k0 + n], p[:, 0:n])
                            k0 += n
                        self.fw.op('dve', lambda e: e.reduce_max(out=st4[:, 0:1], in_=ssb[:, 0:nk], axis=AX.X), ins=[ssb[:, 0:nk]], outs=[st4[:, 0:1]])
                        self.ts(st4[:, 1:2], st4[:, 0:1], -ATTN_SCALE, None, ALU.mult)
                        self.act(pe_[:, 0:nk], ssb[:, 0:nk], AF.Exp, bias=st4[:, 1:2], scale=ATTN_SCALE, accum=st4[:, 2:3])
                        self.recip(st4[:, 3:4], st4[:, 2:3])
                        nkb = nk // 128
                        for g in range((nkb + 3) // 4):
                            nb4 = min(4, nkb - g * 4)
                            p = self.ps()
                            for q in range(nb4):
                                kb = g * 4 + q
                                self.tr(p[:, q * 128:(q + 1) * 128], pe_[:, kb * 128:(kb + 1) * 128], self.ident[:])
                            self.cp('dve' if g % 2 == 0 else 'act', pT[:, g * 4:g * 4 + nb4, :],
                                    p[:, 0:nb4 * 128].rearrange("p (q t) -> p q t", q=nb4))
                        p = self.ps()
                        for kb in range(nkb):
                            self.mm(p[:, 0:64], pT[:, kb, :], Vat[:, kb, h * 64:(h + 1) * 64], kb == 0, kb == nkb - 1)
                        self.ts(osb[:, h * 64:(h + 1) * 64], p[:, 0:64], st4[:, 3:4], None, ALU.mult)
                    p = self.ps()
                    for c2 in range(2):
                        self.tr(p[:, c2 * 128:(c2 + 1) * 128], osb[:, c2 * 128:(c2 + 1) * 128], self.ident[:])
                    self.cp('act', mixT[:, 4:6, ob * 128:(ob + 1) * 128], p[:, 0:256].rearrange("p (c t) -> p c t", c=2))
                self.fw.barrier()

    def pass_sconv(self, L, mixT, P256):
        io = self.io
        NZ = 2308
        with ExitStack() as st:
            ws = self.load_win(st, "wsc", C_SX, 768, L)
            z = self.sb(st, "zsc", [128, 2, NZ], F32)
            sbb = self.sb(st, "sbb", [128, 2, NOUT], F32)
            y = self.sb(st, "ysc", [128, NZ], F32)
            sxs = self.sb(st, "sxs", [128, 512], F32)
            aTg = [self.sb(st, "aTg%d" % i, [128, 8, 512], BF16) for i in range(2)]
            ah = self.sb(st, "ah", [128, 8, 4], BF16)
            zh = self.sb(st, "zh", [128, 2, 4], F32)
            for cc in range(2):
                self.memset('pool', z[:, cc, 0:1], 0.0)
                self.memset('pool', z[:, cc, 257:258], 0.0)
            zoff = {0: 1}
            for i in range(4):
                zoff[OWN0 + 512 * i] = 259 + 512 * i
            for gi, (t0, n, o0) in enumerate(self.OGROUPS):
                a = aTg[gi % 2]
                self.fw.dma('sp', a[:, :, 0:n], self.aT_d[:, :, t0:t0 + n])
                for cc in range(2):
                    p = self.ps()
                    self.proj(p[:, 0:n], ws, cc * 128, 128, a, n)
                    self.cp('act', sxs[:, 0:n], p[:, 0:n])
                    p2 = self.ps()
                    self.proj(p2[:, 0:n], ws, 512 + cc * 128, 128, a, n)
                    self.tt(z[:, cc, zoff[t0]:zoff[t0] + n], sxs[:, 0:n], p2[:, 0:n], ALU.mult)
                    p3 = self.ps()
                    self.proj(p3[:, 0:n], ws, 256 + cc * 128, 128, a, n)
                    self.cp('act', sbb[:, cc, o0:o0 + n], p3[:, 0:n])
            self.fw.dma('sp', ah[:, :, 0:2], self.aT_d[:, :, 256:258])
            self.fw.dma('sp', ah[:, :, 2:4], self.aT_d[:, :, 2302:2304])
            m0, m1 = self.msk[:, 0:1], self.msk[:, 1:2]
            for cc in range(2):
                p = self.ps()
                self.proj(p[:, 0:4], ws, cc * 128, 128, ah, 4)
                self.cp('act', sxs[:, 0:4], p[:, 0:4])
                p2 = self.ps()
                self.proj(p2[:, 0:4], ws, 512 + cc * 128, 128, ah, 4)
                self.tt(zh[:, cc, :], sxs[:, 0:4], p2[:, 0:4], ALU.mult)
                self.ts(z[:, cc, 258:259], zh[:, cc, 3:4], m1, None, ALU.mult)
                self.ts(z[:, cc, 2307:2308], zh[:, cc, 0:1], m0, None, ALU.mult)
            for cc in range(2):
                self.ts(y[:, 1:NZ - 1], z[:, cc, 0:NZ - 2], P256[:, cc, 16:17], P256[:, cc, 19:20], ALU.mult, ALU.add)
                self.stt(y[:, 1:NZ - 1], z[:, cc, 1:NZ - 1], P256[:, cc, 17:18], y[:, 1:NZ - 1], ALU.mult, ALU.add)
                self.stt(y[:, 1:NZ - 1], z[:, cc, 2:NZ], P256[:, cc, 18:19], y[:, 1:NZ - 1], ALU.mult, ALU.add)
                self.tt(mixT[:, 6 + cc, 0:256], y[:, 1:257], sbb[:, cc, 0:256], ALU.mult)
                self.tt(mixT[:, 6 + cc, 256:NOUT], y[:, 259:2307], sbb[:, cc, 256:NOUT], ALU.mult)
            self.fw.barrier()

    def out_glob_block(self, ob):
        return ob if ob < 2 else ob + 16

    def stage_wout(self, L, mixT, hsrc):
        io = self.io
        with ExitStack() as st:
            wo = self.sb(st, "wo", [128, 8, D], BF16)
            self.fw.dma('pool', wo[:], io['w_out'][L].rearrange("(kc p) f -> p kc f", p=128))
            g1 = [self.sb(st, "g1_%d" % j, [128, D], F32) for j in range(2)]
            for j in range(2):
                self.bcload(g1[j][:], self.mod_d[j:j + 1, 2 * D:3 * D])
            tmp = [self.sb(st, "wtmp%d" % i, [128, 512], F32) for i in range(2)]
            hb = [self.sb(st, "whb%d" % i, [128, D], F32) for i in range(2)]
            for ob in range(NOB):
                j = 1 if ob < 2 else 0
                gb = self.out_glob_block(ob)
                h = hb[ob % 2]
                self.fw.dma('sp', h[:], hsrc[gb * 128:(gb + 1) * 128, :])
                for dh in range(2):
                    p = self.ps()
                    for kc in range(8):
                        self.mm(p[:, :], mixT[:, kc, ob * 128:(ob + 1) * 128], wo[:, kc, dh * 512:(dh + 1) * 512], kc == 0, kc == 7)
                    t = tmp[dh]
                    self.tt(t[:], p[:, :], g1[j][:, dh * 512:(dh + 1) * 512], ALU.mult)
                    self.tt(h[:, dh * 512:(dh + 1) * 512], h[:, dh * 512:(dh + 1) * 512], t[:], ALU.add, eng='pool')
                self.fw.dma('sp', self.hmid_d[ob * 128:(ob + 1) * 128, :], h[:])
            self.fw.barrier()

    def stage_norm2(self, L, hnew, fT, G):
        io = self.io
        with ExitStack() as st:
            g2 = [self.sb(st, "g2n_%d" % j, [128, D], F32) for j in range(2)]
            for j in range(2):
                self.bcload(g2[j][:], self.mod_d[j:j + 1, 5 * D:6 * D])
            bd = self.sb(st, "bdown", [NEXP, D], F32)
            self.fw.dma('sp', bd[:], io['exp_b_down'][L])
            GTb = [self.sb(st, "GTb%d" % i, [NEXP, 128], F32) for i in range(2)]
            btmp = [self.sb(st, "btmp%d" % i, [128, 512], F32) for i in range(2)]
            A = [self.sb(st, "A2_%d" % j, [128, D], F32) for j in range(2)]
            S = [self.sb(st, "S2_%d" % j, [128, D], F32) for j in range(2)]
            gb = self.sb(st, "g2n", [128, D], F32)
            self.bcload(gb[:], io['norm2_g'][L:L + 1, :])
            for j in range(2):
                self.bcload(S[j][:], self.mod_d[j:j + 1, 3 * D:4 * D])
                self.bcload(A[j][:], self.mod_d[j:j + 1, 4 * D:5 * D])
                self.ts(A[j][:], A[j][:], 1.0, None, ALU.add)
                self.tt(A[j][:], A[j][:], gb[:], ALU.mult)
            rw = self.sb(st, "rw", [128, 8, NEXP], F32)
            self.fw.dma('sp', rw[:], io['router_w'][L].rearrange("(kc p) e -> p kc e", p=128))
            rb = self.sb(st, "rb", [128, NEXP], F32)
            self.bcload(rb[:], io['router_b'][L:L + 1, :])
            tb = [self.sb(st, "tb%d" % i, [128, D], F32) for i in range(2)]
            junk = self.sb(st, "junk", [128, D], F32)
            ss = [self.sb(st, "ss%d" % i, [128, 1], F32) for i in range(2)]
            f32T = [self.sb(st, "f32T%d" % i, [128, 8, 128], F32) for i in range(2)]
            fbb = [self.sb(st, "fbb%d" % i, [128, 8, 128], BF16) for i in range(2)]
            lg = [self.sb(st, "rlg%d" % i, [128, NEXP], F32) for i in range(2)]
            m8 = [self.sb(st, "m8_%d" % i, [128, 8], F32) for i in range(2)]
            ex = [self.sb(st, "ex%d" % i, [128, NEXP], F32) for i in range(2)]
            mk = [self.sb(st, "mk%d" % i, [128, NEXP], F32) for i in range(2)]
            sc_ = [self.sb(st, "sc%d" % i, [128, 2], F32) for i in range(2)]
            for ob in range(DEBUG.get('n2blocks', NOB)):
                j = 1 if ob < 2 else 0
                i2 = ob % 2
                t, s_, ft = tb[i2], ss[i2], f32T[i2]
                self.act(junk[:], hnew[:, ob, :], AF.Square, accum=s_[:])
                self.act(s_[:], s_[:], AF.Sqrt, bias=EPS, scale=1.0 / D)
                self.recip(s_[:], s_[:])
                self.stt(t[:], hnew[:, ob, :], s_[:, 0:1], A[j][:], ALU.mult, ALU.mult)
                self.tt(t[:], t[:], S[j][:], ALU.add, eng='pool')
                for half in range(2):
                    p = self.ps()
                    for q in range(4):
                        kc = half * 4 + q
                        self.tr(p[:, q * 128:(q + 1) * 128], t[:, kc * 128:(kc + 1) * 128], self.ident[:])
                    src = p[:, :].rearrange("p (q t) -> p q t", q=4)
                    self.cp('act', ft[:, half * 4:(half + 1) * 4, :], src)
                fb_ = fbb[i2]
                self.cp('dve', fb_[:], ft[:])
                self.fw.dma('sp', fT[:, :, ob * 128:(ob + 1) * 128], fb_[:])
                if DEBUG.get('n2', 3) < 2:
                    continue
                p = self.ps()
                for kc in range(8):
                    self.mm(p[:, 0:NEXP], ft[:, kc, :], rw[:, kc, :], kc == 0, kc == 7)
                l_, m_, e_, k_, c_ = lg[i2], m8[i2], ex[i2], mk[i2], sc_[i2]
                self.tt(l_[:], p[:, 0:NEXP], rb[:], ALU.add)
                self.cp('dve', e_[:], l_[:])
                for r_ in range(3):
                    self.fw.op('dve', lambda e: e.reduce_max(out=m_[:, r_:r_ + 1], in_=e_[:], axis=AX.X), ins=[e_[:]], outs=[m_[:, r_:r_ + 1]])
                    self.ts(k_[:], e_[:], m_[:, r_:r_ + 1], -1.0e30, ALU.is_equal, ALU.mult)
                    self.tt(e_[:], e_[:], k_[:], ALU.add)
                self.fw.op('dve', lambda e: e.reduce_max(out=m_[:, 3:4], in_=e_[:], axis=AX.X), ins=[e_[:]], outs=[m_[:, 3:4]])
                self.ts(k_[:], l_[:], m_[:, 3:4], None, ALU.is_ge)
                self.ts(c_[:, 0:1], m_[:, 0:1], -1.0, None, ALU.mult)
                self.act(e_[:], l_[:], AF.Exp, bias=c_[:, 0:1])
                self.tt(e_[:], e_[:], k_[:], ALU.mult)
                self.fw.op('dve', lambda e: e.reduce_sum(out=c_[:, 1:2], in_=e_[:], axis=AX.X), ins=[e_[:]], outs=[c_[:, 1:2]])
                self.recip(c_[:, 1:2], c_[:, 1:2])
                self.ts(G[:, ob, :], e_[:], c_[:, 1:2], None, ALU.mult)
                if DEBUG.get('n2', 3) < 3:
                    continue
                p = self.ps()
                self.tr(p[0:NEXP, 0:128], G[:, ob, :], self.ident[:])
                gt_ = GTb[i2]
                self.cp('act', gt_[:, :], p[0:NEXP, 0:128])
                for dh in range(2):
                    p = self.ps()
                    self.mm(p[:, :], gt_[:, :], bd[:, dh * 512:(dh + 1) * 512], True, True)
                    t2 = btmp[dh]
                    self.tt(t2[:], p[:, :], g2[j][:, dh * 512:(dh + 1) * 512], ALU.mult)
                    self.tt(hnew[:, ob, dh * 512:(dh + 1) * 512], hnew[:, ob, dh * 512:(dh + 1) * 512], t2[:], ALU.add, eng='pool')
            self.fw.barrier()

    def stage_moe(self, L, hnew, fT, G, st):
        io = self.io
        nexp = DEBUG.get('nexp', NEXP)
        g2 = [self.sb(st, "g2_%d" % j, [128, D], F32) for j in range(2)]
        for j in range(2):
            self.bcload(g2[j][:], self.mod_d[j:j + 1, 5 * D:6 * D])
        bgu = self.colload(st, "bgu", [io['exp_b_gu'][L]], 2048)
        tmp = [self.sb(st, "mtmp%d" % i, [128, 512], F32) for i in range(2)]
        ntmp = 0
        wgu = [self.sb(st, "wgu%d" % i, [128, 8, 2, 512], BF16) for i in range(2)]
        wd = [self.sb(st, "wd%d" % i, [128, 4, D], BF16) for i in range(2)]
        actT = [self.sb(st, "actT%d" % i, [128, 4, 512], BF16) for i in range(2)]
        gs = [self.sb(st, "gs%d" % i, [128, 512], F32) for i in range(2)]
        sg = [self.sb(st, "sg%d" % i, [128, 512], F32) for i in range(2)]
        us = [self.sb(st, "us%d" % i, [128, 512], F32) for i in range(2)]
        TG = [(i * 512, 512) for i in range(4)] + [(2048, 256)]
        un = 0
        ng = 0
        for e in range(nexp):
            wsrc = io['exp_w_gu'][L, e].rearrange("(kc p) f -> p kc f", p=128)
            dsrc = io['exp_w_down'][L, e].rearrange("(fc p) d -> p fc d", p=128)
            for u in range(2):
                wg_, wd_ = wgu[un % 2], wd[un % 2]
                un += 1
                self.fw.dma('pool', wg_[:, :, 0, :], wsrc[:, :, u * 512:(u + 1) * 512])
                self.fw.dma('pool', wg_[:, :, 1, :], wsrc[:, :, 1024 + u * 512:1024 + (u + 1) * 512])
                self.fw.dma('pool', wd_[:], dsrc[:, u * 4:(u + 1) * 4, :])
                for (o0, n) in TG:
                    at = actT[ng % 2]
                    ng += 1
                    for fc in range(4):
                        i2 = fc % 2
                        fidx = u * 4 + fc
                        pg = self.ps()
                        for kc in range(8):
                            self.mm(pg[:, 0:n], wg_[:, kc, 0, fc * 128:(fc + 1) * 128], fT[:, kc, o0:o0 + n], kc == 0, kc == 7)
                        pu = self.ps()
                        for kc in range(8):
                            self.mm(pu[:, 0:n], wg_[:, kc, 1, fc * 128:(fc + 1) * 128], fT[:, kc, o0:o0 + n], kc == 0, kc == 7)
                        g_, s_, u_ = gs[i2], sg[i2], us[i2]
                        self.ts(g_[:, 0:n], pg[:, 0:n], bgu[:, fidx, e:e + 1], 7.0, ALU.add, ALU.min)
                        self.act(s_[:, 0:n], g_[:, 0:n], AF.Sigmoid, scale=1.702)
                        self.ts(u_[:, 0:n], pu[:, 0:n], bgu[:, 8 + fidx, e:e + 1], 7.0, ALU.add, ALU.min)
                        self.ts(u_[:, 0:n], u_[:, 0:n], -7.0, 1.0, ALU.max, ALU.add, eng='pool')
                        self.tt(g_[:, 0:n], g_[:, 0:n], s_[:, 0:n], ALU.mult, eng='pool')
                        self.tt(at[:, fc, 0:n], g_[:, 0:n], u_[:, 0:n], ALU.mult)
                    for b in range(n // 128):
                        ob = o0 // 128 + b
                        j = 1 if ob < 2 else 0
                        for dh in range(2):
                            p = self.ps()
                            for fc in range(4):
                                self.mm(p[:, :], at[:, fc, b * 128:(b + 1) * 128], wd_[:, fc, dh * 512:(dh + 1) * 512], fc == 0, fc == 3)
                            t = tmp[ntmp % 2]
                            ntmp += 1
                            self.stt(t[:], p[:, :], G[:, ob, e:e + 1], g2[j][:, dh * 512:(dh + 1) * 512], ALU.mult, ALU.mult)
                            self.tt(hnew[:, ob, dh * 512:(dh + 1) * 512], hnew[:, ob, dh * 512:(dh + 1) * 512], t[:], ALU.add, eng='pool')
        self.fw.barrier()

    def stage_out(self, hnew, outmap, yout):
        io = self.io
        with ExitStack() as st:
            fg = self.sb(st, "fgb", [128, D], F32)
            self.bcload(fg[:], io['final_g'][0:1, :])
            junk = self.sb(st, "junk", [128, D], F32)
            yb = [self.sb(st, "yb%d" % i, [128, D], F32) for i in range(2)]
            ss = [self.sb(st, "ss%d" % i, [128, 1], F32) for i in range(2)]
            for ob in range(NOB):
                dst, is_out = outmap(ob)
                if dst is not None:
                    self.fw.dma('sp', dst, hnew[:, ob, :], is_output=is_out)
                if ob >= 2 and yout is not None:
                    s_, y_ = ss[ob % 2], yb[ob % 2]
                    self.act(junk[:], hnew[:, ob, :], AF.Square, accum=s_[:])
                    self.act(s_[:], s_[:], AF.Sqrt, bias=EPS, scale=1.0 / D)
                    self.recip(s_[:], s_[:])
                    self.stt(y_[:], hnew[:, ob, :], s_[:, 0:1], fg[:], ALU.mult, ALU.mult)
                    self.fw.dma('sp', yout[(ob - 2) * 128:(ob - 1) * 128, :], y_[:], is_output=True)
            self.fw.barrier()

    def layer(self, L, hsrc, outmap, yout, dbg=None):
        io = self.io
        stop = DEBUG.get('stop', '')
        self.adaln(L)
        if stop == 'adaln':
            return
        self.pass0(L, hsrc)
        if stop == 'pass0':
            return
        with ExitStack() as st:
            mixT = self.sb(st, "mixT", [128, 8, NOUT], BF16)
            P256 = self.colload(st, "P256", [
                io['lru_conv_w'][L].rearrange("d k c -> (d k) c"), io['lru_conv_b'][L], io['lru_b_a'][L], io['lru_b_i'][L],
                io['lru_lambda'][L], io['sc_conv_w'][L], io['sc_conv_b'][L:L + 1, :], io['mla_q_norm_g'][L:L + 1, :]], 256)
            skip = DEBUG.get('skip', ())
            if 'fnet' not in skip:
                self.pass_fnet(L, mixT)
            if 'lru' not in skip:
                self.pass_lru(L, mixT, P256)
            if 'attn' not in skip:
                self.pass_attn(L, mixT, P256)
            if 'sconv' not in skip:
                self.pass_sconv(L, mixT, P256)
            if dbg is not None and 'mixT' in dbg:
                self.fw.dma('sp', dbg['mixT'][:, :, :], mixT[:], is_output=True)
            if stop == 'mix':
                return
            self.stage_wout(L, mixT, hsrc)
        with ExitStack() as s2:
            hnew = self.sb(s2, "hnew", [128, NOB, D], F32)
            for ob in range(NOB):
                self.fw.dma('sp' if ob % 2 else 'act', hnew[:, ob, :], self.hmid_d[ob * 128:(ob + 1) * 128, :])
            if dbg is not None and 'hmid' in dbg:
                for ob in range(NOB):
                    self.fw.dma('sp', dbg['hmid'][ob * 128:(ob + 1) * 128, :], hnew[:, ob, :], is_output=True)
            if stop == 'wout':
                return
            fT = self.sb(s2, "fT", [128, 8, NOUT], BF16)
            G = self.sb(s2, "G", [128, NOB, NEXP], F32)
            self.stage_norm2(L, hnew, fT, G)
            if dbg is not None and 'G' in dbg:
                self.fw.dma('sp', dbg['G'].rearrange("(ob p) e -> p ob e", p=128), G[:], is_output=True)
            if stop == 'norm2':
                return
            if 'moe' not in DEBUG.get('skip', ()):
                with ExitStack() as s3:
                    self.stage_moe(L, hnew, fT, G, s3)
            self.stage_out(hnew, outmap, yout)


IN_SPECS = [
    ('hfull', [NTOK, D], F32), ('cvec', [2, D], F32),
    ('mod_w', [1, D, 6 * D], F32), ('mod_b', [1, 6 * D], F32),
    ('norm1_g', [1, D], F32), ('norm2_g', [1, D], F32), ('final_g', [1, D], F32),
    ('w_in', [1, D, 1952], F32),
    ('lru_conv_w', [1, 2, 4, 256], F32), ('lru_conv_b', [1, 2, 256], F32),
    ('lru_w_a', [1, 2, 4, 64, 64], F32), ('lru_b_a', [1, 2, 256], F32),
    ('lru_w_i', [1, 2, 4, 64, 64], F32), ('lru_b_i', [1, 2, 256], F32), ('lru_lambda', [1, 2, 256], F32),
    ('mla_q_norm_g', [1, 256], F32), ('mla_w_uq', [1, 256, 384], F32),
    ('mla_kv_norm_g', [1, 128], F32), ('mla_w_ukv', [1, 128, 512], F32),
    ('sc_conv_w', [1, 3, 256], F32), ('sc_conv_b', [1, 256], F32),
    ('w_out', [1, D, D], F32), ('router_w', [1, D, NEXP], F32), ('router_b', [1, NEXP], F32),
    ('exp_w_gu', [1, NEXP, D, 2 * D], F32), ('exp_b_gu', [1, NEXP, 2 * D], F32),
    ('exp_w_down', [1, NEXP, D, D], F32), ('exp_b_down', [1, NEXP, D], F32),
    ('ident', [128, 128], F32), ('msk', [128, 8], F32), ('prot', [96, 96], F32),
    ('cs64', [256, 512], BF16), ('dft256', [2, 256, 256], BF16),
    ('dftc', [4, 128, 32, 512], BF16), ('dfts', [4, 128, 32, 512], BF16),
    ('ropek_c', [96, NTOK], F32), ('ropek_s', [96, NTOK], F32),
    ('ropeq_c', [96, NOUT], F32), ('ropeq_s', [96, NOUT], F32),
]


def build_layer_program(dbg_outs=()):
    nc = bass.Bass("TRN2", target_bir_lowering=False)
    io = {}
    for name, shape, dt in IN_SPECS:
        io[name] = nc.dram_tensor(name, shape, dt, kind="ExternalInput").ap()
    hout = nc.dram_tensor("hout", [NOUT, D], F32, kind="ExternalOutput").ap()
    yout = nc.dram_tensor("yout", [2048, D], F32, kind="ExternalOutput").ap()
    dbg = {}
    if 'mixT' in dbg_outs:
        dbg['mixT'] = nc.dram_tensor("d_mixT", [128, 8, NOUT], BF16, kind="ExternalOutput").ap()
    if 'hmid' in dbg_outs:
        dbg['hmid'] = nc.dram_tensor("d_hmid", [NOUT, D], F32, kind="ExternalOutput").ap()
    if 'G' in dbg_outs:
        dbg['G'] = nc.dram_tensor("d_G", [NOUT, NEXP], F32, kind="ExternalOutput").ap()
    with ExitStack() as st:
        fw = Fw(nc, st)
        fw.readonly = set(n for n, _, _ in IN_SPECS)
        em = Emit(nc, fw, io)
        em.setup(st)
        em.layer(0, io['hfull'], lambda ob: (hout[ob * 128:(ob + 1) * 128, :], True), yout, dbg if dbg else None)
        fw.finish()
        print("program: instructions=%d waits=%d" % (fw.n_ins, fw.n_wait))
    return nc


def _const_tables(half):
    bf = ml_dtypes.bfloat16
    t = {}
    t['ident'] = np.eye(128, dtype=np.float32)
    m1 = float(half)
    m0 = 1.0 - m1
    mfwd, mbwd = m1, m0
    msk = np.zeros((128, 8), np.float32)
    msk[:, 0], msk[:, 1], msk[:, 2], msk[:, 3], msk[:, 4], msk[:, 5] = m0, m1, mfwd, mbwd, 1 - mfwd, 1 - mbwd
    t['msk'] = msk
    P = np.zeros((96, 96), np.float32)
    for base in (64, 80):
        for i in range(8):
            P[base + 8 + i, base + i] = -1.0
            P[base + i, base + 8 + i] = 1.0
    t['prot'] = P
    j = np.arange(64)
    ang = 2 * np.pi * np.outer(j, j) / 64.0
    C64, S64 = np.cos(ang) / 8.0, np.sin(ang) / 8.0
    cs = np.zeros((256, 512), np.float64)
    for g in range(4):
        cs[g * 64:(g + 1) * 64, g * 64:(g + 1) * 64] = C64
        cs[g * 64:(g + 1) * 64, 256 + g * 64:256 + (g + 1) * 64] = -S64
    t['cs64'] = cs.astype(bf)
    n = np.arange(256)
    a2 = 2 * np.pi * (np.outer(n, n) % 256) / 256.0
    t['dft256'] = np.stack([np.cos(a2) / 16.0, np.sin(a2) / 16.0]).astype(bf)
    own = np.arange(half * 2048, (half + 1) * 2048)
    par = np.arange((1 - half) * 2048, (2 - half) * 2048)
    nn = np.concatenate([par, own]).astype(np.int64)
    kn = (nn[:, None] * own[None, :]) % 4096
    a4 = 2 * np.pi * kn / 4096.0
    for nm, tab in (('dftc', np.cos(a4) / 64.0), ('dfts', np.sin(a4) / 64.0)):
        tab = tab.astype(bf).reshape(32, 128, 4, 512).transpose(2, 1, 0, 3)
        t[nm] = np.ascontiguousarray(tab)
    inv = (10000.0 ** (-np.arange(0, 16, 2, dtype=np.float32) / 16.0)).astype(np.float32)

    def tabs(pos_list, is_ctx):
        npos = len(pos_list)
        c = np.ones((96, npos), np.float32)
        s = np.zeros((96, npos), np.float32)
        pos = np.asarray(pos_list, np.float32)
        row = np.floor(pos / 64.0).astype(np.float32)
        col = (pos - row * 64).astype(np.float32)
        for base, pp in ((64, row), (80, col)):
            angp = pp[None, :] * inv[:, None]
            cc, ss = np.cos(angp).astype(np.float32), np.sin(angp).astype(np.float32)
            for hh in range(2):
                c[base + hh * 8:base + hh * 8 + 8, :] = np.where(is_ctx[None, :], 1.0, cc)
                s[base + hh * 8:base + hh * 8 + 8, :] = np.where(is_ctx[None, :], 0.0, ss)
        return c, s
    kpos = np.concatenate([np.zeros(256), par, own])
    kctx = np.concatenate([np.ones(256, bool), np.zeros(4096, bool)])
    t['ropek_c'], t['ropek_s'] = tabs(kpos, kctx)
    qpos = np.concatenate([np.zeros(256), own])
    qctx = np.concatenate([np.ones(256, bool), np.zeros(2048, bool)])
    t['ropeq_c'], t['ropeq_s'] = tabs(qpos, qctx)
    return t


_TABLES = {}


def core_inputs(core, L, h_lat, h_ctx, inp):
    b, half = core // 2, core % 2
    if half not in _TABLES:
        _TABLES[half] = _const_tables(half)
    m = dict(_TABLES[half])
    own = h_lat[b, half * 2048:(half + 1) * 2048]
    par = h_lat[b, (1 - half) * 2048:(2 - half) * 2048]
    m['hfull'] = np.ascontiguousarray(np.concatenate([h_ctx[b], par, own], axis=0), dtype=np.float32)
    m['cvec'] = np.ascontiguousarray(np.stack([inp['c'][b], inp['c_ctx']]), dtype=np.float32)
    for k in ('mod_w', 'mod_b', 'norm1_g', 'norm2_g', 'w_in', 'lru_conv_w', 'lru_conv_b', 'lru_w_a', 'lru_b_a', 'lru_w_i',
              'lru_b_i', 'lru_lambda', 'mla_q_norm_g', 'mla_w_uq', 'mla_kv_norm_g', 'mla_w_ukv', 'sc_conv_w', 'sc_conv_b',
              'w_out', 'router_w', 'router_b', 'exp_w_gu', 'exp_b_gu', 'exp_w_down', 'exp_b_down'):
        m[k] = np.ascontiguousarray(np.asarray(inp[k])[L:L + 1], dtype=np.float32)
    m['final_g'] = np.ascontiguousarray(np.asarray(inp['final_g'])[None, :], dtype=np.float32)
    return m


_NC = {}

_W2 = ('mod_w', 'mod_b', 'norm1_g', 'norm2_g', 'w_in', 'lru_conv_w', 'lru_conv_b', 'lru_w_a', 'lru_b_a', 'lru_w_i', 'lru_b_i',
       'lru_lambda', 'mla_q_norm_g', 'mla_w_uq', 'mla_kv_norm_g', 'mla_w_ukv', 'sc_conv_w', 'sc_conv_b', 'w_out', 'router_w',
       'router_b', 'exp_w_gu', 'exp_b_gu', 'exp_w_down', 'exp_b_down')
_TABS = ('msk', 'dftc', 'dfts', 'ropek_c', 'ropek_s', 'ropeq_c', 'ropeq_s')


def build_fused_program():
    nc = bass.Bass("TRN2", target_bir_lowering=False)
    io = {}
    for name, shape, dt in IN_SPECS:
        if name == 'hfull':
            for nm in ('hfullA', 'hfullB'):
                io[nm] = nc.dram_tensor(nm, shape, dt, kind="ExternalInput").ap()
            continue
        if name in _W2:
            shape = [2] + list(shape[1:])
        io[name] = nc.dram_tensor(name, shape, dt, kind="ExternalInput").ap()
        if name in _TABS:
            io[name + '_B'] = nc.dram_tensor(name + '_B', shape, dt, kind="ExternalInput").ap()
    yout = nc.dram_tensor("yout", [2048, D], F32, kind="ExternalOutput").ap()
    h1 = nc.dram_tensor("h1_d", [NTOK, D], F32, kind="Internal").ap()
    with ExitStack() as st:
        fw = Fw(nc, st)
        fw.readonly = set(io.keys())
        em = Emit(nc, fw, io)
        em.setup(st)

        def outA(ob):
            gb = ob if ob < 2 else ob + 16
            return h1[gb * 128:(gb + 1) * 128, :], False

        def outB(ob):
            if ob < 2:
                return None, False
            return h1[ob * 128:(ob + 1) * 128, :], False

        em.tsuf = ''
        em.layer(0, io['hfullA'], outA, None)
        em.tsuf = '_B'
        em.layer(0, io['hfullB'], outB, None)
        em.tsuf = ''
        em.layer(1, h1, lambda ob: (None, False), yout)
        fw.finish()
        print("program: instructions=%d waits=%d" % (fw.n_ins, fw.n_wait))
    return nc


def fused_inputs(core, inp):
    b, half = core // 2, core % 2
    for hh in (0, 1):
        if hh not in _TABLES:
            _TABLES[hh] = _const_tables(hh)
    m = dict(_TABLES[half])
    for k in _TABS:
        m[k + '_B'] = _TABLES[1 - half][k]
    x, ctx = inp['x'], inp['ctx']
    own = x[b, half * 2048:(half + 1) * 2048]
    par = x[b, (1 - half) * 2048:(2 - half) * 2048]
    m['hfullA'] = np.ascontiguousarray(np.concatenate([ctx[b], par, own], axis=0), dtype=np.float32)
    m['hfullB'] = np.ascontiguousarray(np.concatenate([ctx[b], own, par], axis=0), dtype=np.float32)
    m['cvec'] = np.ascontiguousarray(np.stack([inp['c'][b], inp['c_ctx']]), dtype=np.float32)
    for k in _W2:
        m[k] = np.ascontiguousarray(inp[k], dtype=np.float32)
    m['final_g'] = np.ascontiguousarray(np.asarray(inp['final_g'])[None, :], dtype=np.float32)
    return m


def kernel(**inputs):
    inp = {k: np.asarray(v) for k, v in inputs.items()}
    if 'nc' not in _NC:
        _NC['nc'] = build_fused_program()
    nc = _NC['nc']
    in_maps = [fused_inputs(c, inp) for c in range(8)]
    res = run_bass_kernel_spmd(nc, in_maps, core_ids=list(range(8)))
    y = np.empty((4, 4096, D), np.float32)
    for c in range(8):
        b, half = c // 2, c % 2
        y[b, half * 2048:(half + 1) * 2048] = res.results[c]['yout']
    return y
```
